# Optimizing a Trainium2 kernel written in Bass

```python
import math
import jax, jax.numpy as jnp
from jax import lax
import numpy as np

D_MODEL = 4096
BATCH = 1
SEQ = 8192
DEPTH = 4

CHUNK = 64
CONV_CH = D_MODEL // 2
CONV_K = 31
SSM_HEAD_DIM = 64
D_INNER = D_MODEL // 2
SSM_HEADS = D_INNER // SSM_HEAD_DIM
SSM_GROUPS = 4
D_STATE = 128
SSM_CONV_K = 4
XBC_DIM = D_INNER + 2 * SSM_GROUPS * D_STATE
DT_MIN = 0.001
DT_MAX = 0.1
DT_PROJ_SCALE = 0.1
N_BRANCHES = 2
SPLITS = (2 * CONV_CH,
          2 * CONV_CH + D_INNER,
          2 * CONV_CH + D_INNER + XBC_DIM,
          2 * CONV_CH + D_INNER + XBC_DIM + SSM_HEADS)
IN_PROJ_DIM = SPLITS[3] + N_BRANCHES * D_MODEL
D_FF_DENSE = 3 * D_MODEL // 2
N_EXPERTS = 8
TOP_K = 2
D_FF_EXPERT = 3 * D_MODEL // 8
N_DENSE = (DEPTH + 1) // 2
N_MOE = DEPTH // 2
DEEPNORM_ALPHA = (2.0 * DEPTH) ** 0.25
DEEPNORM_BETA = (8.0 * DEPTH) ** -0.25
LN_EPS = 1e-5

kernel_name = "hybrid_conformer_ssd_moe_deepnorm"


def layer_norm(x, g, b):
    xf = x.astype(jnp.float32)
    mu = jnp.mean(xf, axis=-1, keepdims=True)
    var = jnp.mean(jnp.square(xf - mu), axis=-1, keepdims=True)
    return ((xf - mu) * lax.rsqrt(var + LN_EPS)).astype(x.dtype) * g + b


def causal_depthwise_conv(x, w):
    k, c = w.shape
    return lax.conv_general_dilated(
        x, w[:, None, :], window_strides=(1,), padding=[(k - 1, 0)],
        dimension_numbers=("NWC", "WIO", "NWC"), feature_group_count=c)


def gated_group_rmsnorm(y, z, g):
    b, L, d = y.shape
    h = (y.astype(jnp.float32) * jax.nn.silu(z.astype(jnp.float32)))
    h = h.reshape(b, L, SSM_GROUPS, d // SSM_GROUPS)
    h = h * lax.rsqrt(jnp.mean(h * h, axis=-1, keepdims=True) + LN_EPS)
    return h.reshape(b, L, d).astype(z.dtype) * g


def ssd_chunked(x, dt, A, B, C):
    b, L, H, P = x.shape
    G, N = B.shape[-2:]
    E = H // G
    nc = L // CHUNK
    xs = (x.astype(jnp.float32) * dt[..., None]).reshape(b, nc, CHUNK, G, E, P)
    dA = (dt * A).reshape(b, nc, CHUNK, G, E)
    dA = jnp.moveaxis(dA, 2, -1)
    cs = jnp.cumsum(dA, axis=-1)
    Bc = B.astype(jnp.float32).reshape(b, nc, CHUNK, G, N)
    Cc = C.astype(jnp.float32).reshape(b, nc, CHUNK, G, N)
    causal = jnp.tril(jnp.ones((CHUNK, CHUNK), dtype=bool))
    seg = cs[..., :, None] - cs[..., None, :]
    decay = jnp.exp(jnp.where(causal, seg, -jnp.inf))
    scores = jnp.einsum("bclgn,bcsgn->bcgls", Cc, Bc)
    y_diag = jnp.einsum("bcgls,bcgels,bcsgep->bclgep", scores, decay, xs)
    decay_to_end = jnp.exp(cs[..., -1:] - cs)
    chunk_states = jnp.einsum("bclgn,bcgel,bclgep->bcgepn", Bc, decay_to_end, xs)
    chunk_decay = jnp.exp(cs[..., -1])

    def step(state, inp):
        s_c, a_c = inp
        return state * a_c[..., None, None] + s_c, state

    init = jnp.zeros((b, G, E, P, N), jnp.float32)
    _, prev_states = lax.scan(step, init, (jnp.moveaxis(chunk_states, 1, 0),
                                           jnp.moveaxis(chunk_decay, 1, 0)))
    prev_states = jnp.moveaxis(prev_states, 0, 1)
    y_off = jnp.einsum("bclgn,bcgepn,bcgel->bclgep", Cc, prev_states, jnp.exp(cs))
    return (y_diag + y_off).reshape(b, L, H, P)


def hybrid_mixer(x, w_in, b_gate, conv_dw, conv_ln_g, conv_ln_b, conv_pw2,
                 ssm_conv_w, ssm_conv_b, dt_bias, a_log, d_skip, ssm_norm_g,
                 ssm_out, w_o):
    b, L, _ = x.shape
    proj = x @ w_in
    glu_in, z, xbc, dt, gates = jnp.split(proj, SPLITS, axis=-1)

    u_a, u_g = jnp.split(glu_in, 2, axis=-1)
    u = u_a * jax.nn.sigmoid(u_g)
    u = causal_depthwise_conv(u, conv_dw)
    u = jax.nn.silu(layer_norm(u, conv_ln_g, conv_ln_b))
    conv_branch = u @ conv_pw2

    xbc = jax.nn.silu(causal_depthwise_conv(xbc, ssm_conv_w) + ssm_conv_b)
    xs, Bm, Cm = jnp.split(xbc, (D_INNER, D_INNER + SSM_GROUPS * D_STATE), axis=-1)
    xs = xs.reshape(b, L, SSM_HEADS, SSM_HEAD_DIM)
    Bm = Bm.reshape(b, L, SSM_GROUPS, D_STATE)
    Cm = Cm.reshape(b, L, SSM_GROUPS, D_STATE)
    dt = jax.nn.softplus((dt + dt_bias).astype(jnp.float32))
    A = -jnp.exp(a_log.astype(jnp.float32))
    y = ssd_chunked(xs, dt, A, Bm, Cm) + d_skip[:, None].astype(jnp.float32) * xs
    y = gated_group_rmsnorm(y.reshape(b, L, D_INNER).astype(x.dtype), z, ssm_norm_g)
    ssm_branch = y @ ssm_out

    g_conv, g_ssm = jnp.split(jax.nn.sigmoid(gates + b_gate), N_BRANCHES, axis=-1)
    return (g_conv * conv_branch + g_ssm * ssm_branch) @ w_o


def swiglu(x, w_gate, w_up, w_down):
    return (jax.nn.silu(x @ w_gate) * (x @ w_up)) @ w_down


def moe_swiglu(x, w_router, w_gate, w_up, w_down):
    logits = (x @ w_router).astype(jnp.float32)
    top_vals, top_idx = lax.top_k(logits, TOP_K)
    top_w = jax.nn.softmax(top_vals, axis=-1)
    combine = jnp.sum(jax.nn.one_hot(top_idx, N_EXPERTS, dtype=jnp.float32)
                      * top_w[..., None], axis=-2).astype(x.dtype)
    h = jax.nn.silu(jnp.einsum("bld,edf->eblf", x, w_gate)) \
        * jnp.einsum("bld,edf->eblf", x, w_up)
    h = h * jnp.moveaxis(combine, -1, 0)[..., None]
    return jnp.einsum("eblf,efd->bld", h, w_down)


def setup_inputs(seed: int = 0) -> dict:
    key = jax.random.key(seed)
    ks = jax.random.split(key, 32)
    f32 = jnp.float32

    def nrm(k, shape, scale):
        return jax.random.normal(k, shape, f32) * scale

    beta = DEEPNORM_BETA
    col_scale = jnp.ones((IN_PROJ_DIM,), f32).at[SPLITS[2]:SPLITS[3]].set(DT_PROJ_SCALE)
    u = jax.random.uniform(ks[9], (DEPTH, SSM_HEADS), f32)
    dt0 = jnp.maximum(jnp.exp(u * (math.log(DT_MAX) - math.log(DT_MIN)) + math.log(DT_MIN)), 1e-4)
    return {
        "x": nrm(ks[0], (BATCH, SEQ, D_MODEL), 1.0),
        "w_in": nrm(ks[1], (DEPTH, D_MODEL, IN_PROJ_DIM), D_MODEL ** -0.5) * col_scale,
        "b_gate": nrm(ks[2], (DEPTH, N_BRANCHES * D_MODEL), 0.02),
        "conv_dw": nrm(ks[3], (DEPTH, CONV_K, CONV_CH), CONV_K ** -0.5),
        "conv_ln_g": 1.0 + nrm(ks[4], (DEPTH, CONV_CH), 0.02),
        "conv_ln_b": nrm(ks[5], (DEPTH, CONV_CH), 0.02),
        "conv_pw2": nrm(ks[6], (DEPTH, CONV_CH, D_MODEL), beta * CONV_CH ** -0.5),
        "ssm_conv_w": nrm(ks[7], (DEPTH, SSM_CONV_K, XBC_DIM), SSM_CONV_K ** -0.5),
        "ssm_conv_b": nrm(ks[8], (DEPTH, XBC_DIM), 0.02),
        "dt_bias": dt0 + jnp.log(-jnp.expm1(-dt0)),
        "a_log": jnp.log(jax.random.uniform(ks[10], (DEPTH, SSM_HEADS), f32, 1.0, 16.0)),
        "d_skip": 1.0 + nrm(ks[11], (DEPTH, SSM_HEADS), 0.02),
        "ssm_norm_g": 1.0 + nrm(ks[12], (DEPTH, D_INNER), 0.02),
        "ssm_out": nrm(ks[13], (DEPTH, D_INNER, D_MODEL), beta * D_INNER ** -0.5),
        "w_o": nrm(ks[14], (DEPTH, D_MODEL, D_MODEL), beta * D_MODEL ** -0.5),
        "ln_mix_g": 1.0 + nrm(ks[15], (DEPTH, D_MODEL), 0.02),
        "ln_mix_b": nrm(ks[16], (DEPTH, D_MODEL), 0.02),
        "ffn_w_gate": nrm(ks[17], (N_DENSE, D_MODEL, D_FF_DENSE), D_MODEL ** -0.5),
        "ffn_w_up": nrm(ks[18], (N_DENSE, D_MODEL, D_FF_DENSE), D_MODEL ** -0.5),
        "ffn_w_down": nrm(ks[19], (N_DENSE, D_FF_DENSE, D_MODEL), beta * D_FF_DENSE ** -0.5),
        "moe_router": nrm(ks[20], (N_MOE, D_MODEL, N_EXPERTS), D_MODEL ** -0.5),
        "moe_w_gate": nrm(ks[21], (N_MOE, N_EXPERTS, D_MODEL, D_FF_EXPERT), D_MODEL ** -0.5),
        "moe_w_up": nrm(ks[22], (N_MOE, N_EXPERTS, D_MODEL, D_FF_EXPERT), D_MODEL ** -0.5),
        "moe_w_down": nrm(ks[23], (N_MOE, N_EXPERTS, D_FF_EXPERT, D_MODEL), beta * D_FF_EXPERT ** -0.5),
        "ln_ffn_g": 1.0 + nrm(ks[24], (DEPTH, D_MODEL), 0.02),
        "ln_ffn_b": nrm(ks[25], (DEPTH, D_MODEL), 0.02),
    }


def reference(x, w_in, b_gate, conv_dw, conv_ln_g, conv_ln_b, conv_pw2,
              ssm_conv_w, ssm_conv_b, dt_bias, a_log, d_skip, ssm_norm_g,
              ssm_out, w_o, ln_mix_g, ln_mix_b, ffn_w_gate, ffn_w_up,
              ffn_w_down, moe_router, moe_w_gate, moe_w_up, moe_w_down,
              ln_ffn_g, ln_ffn_b):
    for layer in range(DEPTH):
        mix = hybrid_mixer(x, w_in[layer], b_gate[layer], conv_dw[layer],
                           conv_ln_g[layer], conv_ln_b[layer], conv_pw2[layer],
                           ssm_conv_w[layer], ssm_conv_b[layer], dt_bias[layer],
                           a_log[layer], d_skip[layer], ssm_norm_g[layer],
                           ssm_out[layer], w_o[layer])
        x = layer_norm(DEEPNORM_ALPHA * x + mix, ln_mix_g[layer], ln_mix_b[layer])
        i = layer // 2
        if layer % 2 == 0:
            ff = swiglu(x, ffn_w_gate[i], ffn_w_up[i], ffn_w_down[i])
        else:
            ff = moe_swiglu(x, moe_router[i], moe_w_gate[i], moe_w_up[i], moe_w_down[i])
        x = layer_norm(DEEPNORM_ALPHA * x + ff, ln_ffn_g[layer], ln_ffn_b[layer])
    return x
```

```python
import contextlib
import numpy as np
import concourse.bass as bass
import concourse.mybir as mybir
from concourse.bass_utils import run_bass_kernel_spmd

F32 = mybir.dt.float32
BF16 = mybir.dt.bfloat16
AF = mybir.ActivationFunctionType
ALU = mybir.AluOpType
AX = mybir.AxisListType

NCORES = 8
ENGS = ("pe", "act", "dve", "pool", "sp")
NDMASEM = 10
BLK = 1024
LN_EPS = 1e-5


class Cfg:
    def __init__(self, D=4096, SEQ=8192, DEPTH=4, GROUP_MAX=24):
        self.D, self.SEQ, self.DEPTH, self.GROUP_MAX = D, SEQ, DEPTH, GROUP_MAX
        self.T = SEQ // NCORES
        self.TH = min(512, self.T)
        self.NH = self.T // self.TH
        self.NT = self.T // 128
        self.KC = D // 128
        self.CC = D // 2
        self.CCc = self.CC // 128
        self.DI = D // 2
        self.DIc = self.DI // 128
        self.H = self.DI // 64
        self.G = 4
        self.E = self.H // self.G
        self.EW = self.E * 64
        self.EWc = self.EW // 128
        self.N = 128
        self.XBC = self.DI + 2 * self.G * self.N
        self.XBCc = self.XBC // 128
        self.FFD = 3 * D // 2
        self.NE = 8
        self.FFE = 3 * D // 8
        self.FFEc = self.FFE // 128
        self.CK = 31
        self.SK = 4
        self.SPL = (2 * self.CC, 2 * self.CC + self.DI, 2 * self.CC + self.DI + self.XBC,
                    2 * self.CC + self.DI + self.XBC + self.H)
        self.INP = self.SPL[3] + 2 * D
        self.ALPHA = (2.0 * DEPTH) ** 0.25
        self.HF = self.CCc * 30 + self.XBCc * 3
        self.SF = self.G * self.EW + self.H
        o = 0
        self.pp = {}
        for name, n in (("b_gate", 2 * self.KC), ("conv_dw", self.CCc * self.CK), ("conv_ln_g", self.CCc),
                        ("conv_ln_b", self.CCc), ("ssm_conv_w", self.XBCc * self.SK), ("ssm_conv_b", self.XBCc),
                        ("ssm_norm_g", self.DIc), ("ln_mix_g", self.KC), ("ln_mix_b", self.KC),
                        ("ln_ffn_g", self.KC), ("ln_ffn_b", self.KC), ("dt_bias", self.H), ("a_log", self.H),
                        ("d_skip", self.H), ("wr", self.KC * self.NE), ("wdt", self.KC * self.H)):
            self.pp[name] = (o, n)
            o += n
        self.NP = o

    def ffn_groups(self, layer):
        if layer % 2 == 0:
            nch = self.FFD // 128
            return [[("d", None, a, min(a + self.GROUP_MAX, nch))] for a in range(0, nch, self.GROUP_MAX)]
        per = max(1, self.GROUP_MAX // self.FFEc)
        return [[("e", e, 0, self.FFEc) for e in range(a, min(a + per, self.NE))] for a in range(0, self.NE, per)]


def _wl(W):
    K, N = W.shape
    return np.ascontiguousarray(W.reshape(K // 128, 128, N // 128, 128).transpose(2, 1, 0, 3)).reshape(-1)


def _layer_weight_groups(cfg, l, inp):
    w_in = inp["w_in"][l]
    s = cfg.SPL
    CC = cfg.CC
    g0 = [("glu_a", w_in[:, 0:CC]), ("glu_g", w_in[:, CC:2 * CC]), ("xbc", w_in[:, s[1]:s[2]]),
          ("z", w_in[:, s[0]:s[1]]), ("gates", w_in[:, s[3]:])]
    g1 = [("pw2", inp["conv_pw2"][l]), ("ssm_out", inp["ssm_out"][l]), ("w_o", inp["w_o"][l])]
    groups = [g0, g1]
    i = l // 2
    for grp in cfg.ffn_groups(l):
        if grp[0][0] == "d":
            _, _, a, b = grp[0]
            groups.append([("fg", inp["ffn_w_gate"][i][:, a * 128:b * 128]),
                           ("fu", inp["ffn_w_up"][i][:, a * 128:b * 128]),
                           ("fd", inp["ffn_w_down"][i][a * 128:b * 128, :])])
        else:
            es = [e for (_, e, _, _) in grp]
            groups.append([("fg", np.concatenate([inp["moe_w_gate"][i][e] for e in es], axis=1)),
                           ("fu", np.concatenate([inp["moe_w_up"][i][e] for e in es], axis=1)),
                           ("fd", np.concatenate([inp["moe_w_down"][i][e] for e in es], axis=0))])
    return groups


def _group_meta(cfg, l):
    D, CC, DI = cfg.D, cfg.CC, cfg.DI
    g0 = [("glu_a", D, CC), ("glu_g", D, CC), ("xbc", D, cfg.XBC), ("z", D, DI), ("gates", D, 2 * D)]
    g1 = [("pw2", CC, D), ("ssm_out", DI, D), ("w_o", D, D)]
    groups = [g0, g1]
    for grp in cfg.ffn_groups(l):
        F = sum((b - a) * 128 for (_, _, a, b) in grp)
        groups.append([("fg", D, F), ("fu", D, F), ("fd", F, D)])
    return groups


def _pack_params(cfg, l, inp):
    pp = np.zeros((128, cfg.NP), np.float32)

    def put(name, arr):
        o, n = cfg.pp[name]
        pp[:, o:o + n] = arr.reshape(128, n)

    def cols(v):
        return np.ascontiguousarray(v.reshape(-1, 128).T)

    put("b_gate", cols(inp["b_gate"][l]))
    put("conv_dw", np.ascontiguousarray(inp["conv_dw"][l].reshape(cfg.CK, cfg.CCc, 128).transpose(2, 1, 0)))
    put("conv_ln_g", cols(inp["conv_ln_g"][l]))
    put("conv_ln_b", cols(inp["conv_ln_b"][l]))
    put("ssm_conv_w", np.ascontiguousarray(inp["ssm_conv_w"][l].reshape(cfg.SK, cfg.XBCc, 128).transpose(2, 1, 0)))
    put("ssm_conv_b", cols(inp["ssm_conv_b"][l]))
    put("ssm_norm_g", cols(inp["ssm_norm_g"][l]))
    put("ln_mix_g", cols(inp["ln_mix_g"][l]))
    put("ln_mix_b", cols(inp["ln_mix_b"][l]))
    put("ln_ffn_g", cols(inp["ln_ffn_g"][l]))
    put("ln_ffn_b", cols(inp["ln_ffn_b"][l]))
    put("dt_bias", np.broadcast_to(inp["dt_bias"][l][None, :], (128, cfg.H)))
    put("a_log", np.broadcast_to(inp["a_log"][l][None, :], (128, cfg.H)))
    put("d_skip", np.broadcast_to(inp["d_skip"][l][None, :], (128, cfg.H)))
    wdt = inp["w_in"][l][:, cfg.SPL[2]:cfg.SPL[3]]
    put("wdt", np.ascontiguousarray(wdt.reshape(cfg.KC, 128, cfg.H).transpose(1, 0, 2)))
    if l % 2 == 1:
        wr = inp["moe_router"][l // 2]
        put("wr", np.ascontiguousarray(wr.reshape(cfg.KC, 128, cfg.NE).transpose(1, 0, 2)))
    return pp


def _consts():
    k = np.arange(128)
    ident = (k[:, None] == k[None, :]).astype(np.float32)
    ones = np.ones((128, 128), np.float32)
    tri = (k[:, None] <= k[None, :]).astype(np.float32)
    gt = (k[:, None] > k[None, :]).astype(np.float32)
    return np.concatenate([ident, ones, tri, gt], axis=1)


class Buf:
    __slots__ = ("lastw", "rd_eng", "rd_dma")

    def __init__(self):
        self.lastw = None
        self.rd_eng = {}
        self.rd_dma = []


class Op:
    __slots__ = ("eng", "fn", "deps", "sem", "val", "needed", "isdma", "prewait", "inc")

    def __init__(self, eng, fn, isdma, inc):
        self.eng, self.fn, self.isdma, self.inc = eng, fn, isdma, inc
        self.deps = []
        self.sem = None
        self.val = 0
        self.needed = False
        self.prewait = None


class Plan:
    def __init__(self, nc):
        self.nc = nc
        self.ops = {e: [] for e in ENGS}

    def emit(self, eng, fn, reads=(), writes=(), dma=False, inc=None):
        if inc is None:
            inc = 16 if dma else 1
        op = Op(eng, fn, dma, inc)
        deps = {}
        for b in reads:
            if b.lastw is not None:
                deps[id(b.lastw)] = b.lastw
        for b in writes:
            if b.lastw is not None:
                deps[id(b.lastw)] = b.lastw
            for d in b.rd_eng.values():
                deps[id(d)] = d
            for d in b.rd_dma:
                deps[id(d)] = d
        lst = []
        for d in deps.values():
            if (not d.isdma) and (not dma) and d.eng == eng and eng == "pe":
                continue
            lst.append(d)
            d.needed = True
        op.deps = lst
        for b in reads:
            if dma:
                b.rd_dma.append(op)
            else:
                b.rd_eng[eng] = op
        for b in writes:
            b.lastw = op
            b.rd_eng = {}
            b.rd_dma = []
        self.ops[eng].append(op)
        return op

    def finalize(self, stack):
        nc = self.nc
        self.last_dma = {}
        for e in ENGS:
            prog = stack.enter_context(nc.semaphore("prog_" + e))
            has_dma = any(op.isdma for op in self.ops[e])
            dsems = [stack.enter_context(nc.semaphore("dma_%s_%d" % (e, i))) for i in range(NDMASEM)] if has_dma else []
            cnt = 0
            dcount = [0] * NDMASEM
            dlast = [None] * NDMASEM
            rr = 0
            for op in self.ops[e]:
                if op.isdma:
                    k = rr % NDMASEM
                    rr += 1
                    op.prewait = dlast[k]
                    dcount[k] += op.inc
                    op.sem = dsems[k]
                    op.val = dcount[k]
                    dlast[k] = op
                elif op.needed:
                    cnt += 1
                    op.sem = prog
                    op.val = cnt
            self.last_dma[e] = [d for d in dlast if d is not None]

    def replay(self, eng, e):
        waited = {}
        for op in self.ops[eng]:
            ws = list(op.deps)
            if op.prewait is not None:
                ws.append(op.prewait)
            for d in ws:
                key = id(d.sem)
                if waited.get(key, 0) >= d.val:
                    continue
                waited[key] = d.val
                e.wait_ge(d.sem, d.val)
            ins = op.fn(e)
            if op.isdma:
                ins.then_inc(op.sem, op.inc)
            elif op.needed:
                ins.then_inc(op.sem, 1)
        for d in self.last_dma[eng]:
            if waited.get(id(d.sem), 0) < d.val:
                e.wait_ge(d.sem, d.val)

    def run_block(self):
        with self.nc.Block() as block:
            @block.tensor
            def _(e):
                self.replay("pe", e)

            @block.scalar
            def _(e):
                self.replay("act", e)

            @block.vector
            def _(e):
                self.replay("dve", e)

            @block.gpsimd
            def _(e):
                self.replay("pool", e)

            @block.sync
            def _(e):
                self.replay("sp", e)


class V:
    __slots__ = ("ap", "bufs")

    def __init__(self, ap, bufs):
        self.ap = ap
        self.bufs = bufs


class Arena:
    def __init__(self, nc, stack, nbytes):
        self.t = stack.enter_context(nc.sbuf_tensor("arena", [128, nbytes // 2], BF16))
        self.nbytes = nbytes
        self.blocks = [Buf() for _ in range((nbytes + BLK - 1) // BLK)]

    def view(self, off, dims, dtype, part=128):
        es = 4 if dtype == F32 else 2
        n = 1
        for d in dims:
            n *= d
        nb = n * es
        assert off % 4 == 0 and off + nb <= self.nbytes, (off, nb, self.nbytes)
        a = self.t[0:part, off // 2:(off + nb) // 2]
        if dtype == F32:
            a = a.bitcast(F32)
        if len(dims) == 2:
            a = a.rearrange("p (a b) -> p a b", a=dims[0])
        elif len(dims) == 3:
            a = a.rearrange("p (a b c) -> p a b c", a=dims[0], b=dims[1])
        bufs = self.blocks[off // BLK:(off + nb - 1) // BLK + 1]
        return V(a, bufs)


class Tile:
    def __init__(self, arena, off, dims, dtype, part=128):
        self.arena, self.off, self.dims, self.dtype, self.part = arena, off, tuple(dims), dtype, part
        self.es = 4 if dtype == F32 else 2
        n = 1
        for d in dims:
            n *= d
        self.nbytes = n * self.es

    def all(self):
        return self.arena.view(self.off, self.dims, self.dtype, self.part)

    def row(self, a, lo=0, hi=None):
        B = self.dims[-1]
        if hi is None:
            hi = B
        if len(self.dims) == 1:
            assert a == 0
            return self.arena.view(self.off + lo * self.es, [hi - lo], self.dtype, self.part)
        return self.arena.view(self.off + (a * B + lo) * self.es, [hi - lo], self.dtype, self.part)

    def rows(self, a0, a1):
        B = self.dims[-1]
        return self.arena.view(self.off + a0 * B * self.es, [a1 - a0, B], self.dtype, self.part)

    def cols(self, lo, hi):
        v = self.all()
        return V(v.ap[:, :, lo:hi], v.bufs)


def build(cfg, layers=None):
    c = cfg
    if layers is None:
        layers = list(range(c.DEPTH))
    T, TH, NH, NT, KC = c.T, c.TH, c.NH, c.NT, c.KC
    nc = bass.Bass("TRN2", target_bir_lowering=False)
    stack = contextlib.ExitStack()
    P = Plan(nc)

    x_in = nc.dram_tensor("x_in", [KC, 128, T], F32, kind="ExternalInput")
    consts_in = nc.dram_tensor("consts", [128, 512], F32, kind="ExternalInput")
    sels_in = nc.dram_tensor("sels", [128, 16], F32, kind="ExternalInput")
    y_out = nc.dram_tensor("y_out", [KC, 128, T], F32, kind="ExternalOutput")
    pp_in = {}
    wg_in = {}
    wg_sh = {}
    wg_full = {}
    wmeta = {}
    for l in layers:
        pp_in[l] = nc.dram_tensor("pp_%d" % l, [128, c.NP], F32, kind="ExternalInput")
        for gi, mats in enumerate(_group_meta(c, l)):
            tot = sum(K * N for (_, K, N) in mats)
            rows = tot // (NCORES * 2048)
            assert rows * NCORES * 2048 == tot
            wg_in[(l, gi)] = nc.dram_tensor("wg_%d_%d" % (l, gi), [rows, 2048], F32, kind="ExternalInput")
            wg_sh[(l, gi)] = nc.dram_tensor("wsh_%d_%d" % (l, gi), [rows, 2048], BF16)
            wg_full[(l, gi)] = nc.dram_tensor("wfull_%d_%d" % (l, gi), [rows * NCORES, 2048], BF16)
            o = 0
            for (name, K, N) in mats:
                wmeta[(l, gi, name)] = (o, K // 128, N // 128)
                o += K * N

    def scratch(name, nchunks, dtype):
        t = nc.dram_tensor(name, [nchunks, 128, T], dtype)
        return t, [Buf() for _ in range(nchunks)]

    XRES, XRES_b = scratch("xres", KC, F32)
    S, S_b = scratch("sres", KC, F32)
    U, U_b = scratch("u_scr", c.CCc, F32)
    XB, XB_b = scratch("xbc_scr", c.XBCc, F32)
    ZS, ZS_b = scratch("zs_scr", c.DIc, BF16)
    GT, GT_b = scratch("gt_scr", 2 * KC, BF16)
    MRG, MRG_b = scratch("mrg_scr", KC, BF16)
    HSEND = nc.dram_tensor("hsend", [128, c.HF], F32)
    HG = nc.dram_tensor("hgath", [128 * NCORES, c.HF], F32)
    SSEND = nc.dram_tensor("ssend", [128, c.SF], F32)
    SG = nc.dram_tensor("sgath", [128 * NCORES, c.SF], F32)
    HSEND_b, HG_b, SSEND_b, SG_b = Buf(), Buf(), Buf(), Buf()
    XIN_b = [Buf() for _ in range(KC)]
    YOUT_b = [Buf() for _ in range(KC)]
    wfull_b = {k: Buf() for k in wg_full}
    wsh_b = {k: Buf() for k in wg_full}

    SLOT = KC * 128 * 2
    NSLOT = 3
    off = 0

    def alloc(nbytes, align=BLK):
        nonlocal off
        off = (off + align - 1) // align * align
        o = off
        off += nbytes
        return o

    ar_bytes = int(nc.sbuf_bytes_remaining) // 1024 * 1024
    E_, EW_ = c.E, c.EW
    FLW = EW_ + c.H
    ssd1 = [("BFM", c.G * T * 2), ("CFM", c.G * T * 2), ("BTM", NT * c.G * 128 * 2), ("YF", c.EWc * T * 4),
            ("STL", c.G * EW_ * 4), ("STT", EW_ * 4), ("RR", EW_ * 4), ("FL0", FLW * 4), ("FL1", FLW * 4),
            ("STB", EW_ * 2), ("SMK", 128 * 4), ("HA", c.HF * 4)]
    ssd2 = [("XW0", EW_ * 2), ("XW1", EW_ * 2), ("LH0", 2048), ("LH1", 2048), ("DD0", 2048), ("DD1", 2048),
            ("MH0", 1024), ("MH1", 1024), ("YT0", EW_ * 4), ("YT1", EW_ * 4), ("YT2", EW_ * 4)]

    def lay(items, base):
        o = base
        res = {}
        for nm, nb in items:
            o = (o + 255) // 256 * 256
            res[nm] = o
            o += nb
        return res, o - base

    _, n1 = lay(ssd1, 0)
    _, n2 = lay(ssd2, 0)
    XT_o = alloc(max(KC * T * 2, n1))
    OPB_o = alloc(KC * T * 2)
    WRB_o = alloc(max(NSLOT * SLOT, n2))
    WR_o = [WRB_o + i * SLOT for i in range(NSLOT)]
    S1, _ = lay(ssd1, XT_o)
    S2, _ = lay(ssd2, WRB_o)
    CONST_o = alloc(512 * 4)
    IDB_o = alloc(128 * 2, 64)
    SEL_o = alloc(16 * 4, 64)
    NPL = c.pp["wdt"][0]
    PP_o = alloc(NPL * 4, 64)
    PPB_o = alloc(KC * c.H * 2, 64)
    TW = T + 32
    TF_o = [alloc(TW * 4, 256) for _ in range(4)]
    TB_o = [alloc(T * 2, 256) for _ in range(4)]
    RS_o = alloc(T * 4, 256)
    NM_o = alloc(T * 4, 256)
    HA_o = S1["HA"]
    HL_o = [TF_o[2], TF_o[3]]
    SM_o = {}
    for nm in ("DT", "DA", "CS", "TOT", "E1", "W2", "ACH", "TMP", "TMP2"):
        SM_o[nm] = alloc(NT * c.H * 4, 64)
    AN_o = alloc(c.H * 4, 64)
    LT_o = alloc(c.H * 4, 64)
    RT_o = {}
    for nm in ("LT", "EQ", "L2", "EX", "CMB"):
        RT_o[nm] = alloc(NT * c.NE * 4, 64)
    RM_o = {nm: alloc(NT * 4, 64) for nm in ("M1", "M2", "DEN")}
    LG_o = TF_o[3]
    DG_o = [alloc(128 * 4, 64) for _ in range(2)]
    assert off <= ar_bytes, ("SBUF over budget", off, ar_bytes)
    arena = Arena(nc, stack, ar_bytes)

    XT = Tile(arena, XT_o, [KC, T], BF16)
    OPB = Tile(arena, OPB_o, [KC, T], BF16)
    WR = [Tile(arena, o, [KC * 128], BF16) for o in WR_o]
    CONST = Tile(arena, CONST_o, [4, 128], F32)
    IDB = Tile(arena, IDB_o, [128], BF16)
    SEL = Tile(arena, SEL_o, [16], F32)
    PPt = Tile(arena, PP_o, [NPL], F32)
    WDTB = Tile(arena, PPB_o, [KC, c.H], BF16)
    TF = [Tile(arena, o, [TW], F32) for o in TF_o]
    TB = [Tile(arena, o, [T], BF16) for o in TB_o]
    RSTD = Tile(arena, RS_o, [T], F32)
    NMR = Tile(arena, NM_o, [T], F32)
    HA = Tile(arena, HA_o, [c.HF], F32)
    HL = [Tile(arena, o, [c.HF], F32) for o in HL_o]
    SMt = {nm: Tile(arena, o, [NT, c.H], F32) for nm, o in SM_o.items()}
    AN = Tile(arena, AN_o, [c.H], F32)
    LTOT = Tile(arena, LT_o, [c.H], F32)
    RTt = {nm: Tile(arena, o, [NT, c.NE], F32) for nm, o in RT_o.items()}
    RMt = {nm: Tile(arena, o, [NT], F32) for nm, o in RM_o.items()}
    LG = Tile(arena, LG_o, [T], F32, part=c.NE)
    DG = [Tile(arena, o, [128], F32) for o in DG_o]
    IDENT = CONST.row(0)
    ONES = CONST.row(1)
    TRI = CONST.row(2)
    GTM = CONST.row(3)

    BFM = Tile(arena, S1["BFM"], [c.G, T], BF16)
    CFM = Tile(arena, S1["CFM"], [c.G, T], BF16)
    BTM = Tile(arena, S1["BTM"], [NT, c.G * 128], BF16)
    YF = Tile(arena, S1["YF"], [c.EWc, T], F32)
    STT = Tile(arena, S1["STT"], [c.EW], F32)
    STL = Tile(arena, S1["STL"], [c.G, c.EW], F32)
    RR = Tile(arena, S1["RR"], [c.EW], F32)
    FL = [Tile(arena, S1["FL0"], [FLW], F32), Tile(arena, S1["FL1"], [FLW], F32)]
    STB = Tile(arena, S1["STB"], [c.EW], BF16)
    SMK = Tile(arena, S1["SMK"], [128], F32)
    XW = [Tile(arena, S2["XW0"], [c.EW], BF16), Tile(arena, S2["XW1"], [c.EW], BF16)]
    LH = [Tile(arena, S2["LH0"], [4, 128], F32), Tile(arena, S2["LH1"], [4, 128], F32)]
    DD = [Tile(arena, S2["DD0"], [4, 128], F32), Tile(arena, S2["DD1"], [4, 128], F32)]
    MH = [Tile(arena, S2["MH0"], [4, 128], BF16), Tile(arena, S2["MH1"], [4, 128], BF16)]
    YT = [Tile(arena, S2["YT0"], [c.EW], F32), Tile(arena, S2["YT1"], [c.EW], F32)]
    YT2 = Tile(arena, S2["YT2"], [c.EW], F32)
    XSTM = Tile(arena, OPB_o, [NT, c.DI], BF16)

    psum = [stack.enter_context(nc.psum_tensor("ps%d" % i, [128, 512], F32)) for i in range(8)]
    ps_b = [Buf() for _ in range(8)]
    reserved = set()
    rr_state = [0]

    def bank():
        while True:
            b = rr_state[0] % 8
            rr_state[0] += 1
            if b not in reserved:
                return b

    def PS(b, lo=0, hi=512, part=128):
        return V(psum[b][0:part, lo:hi], [ps_b[b]])

    def PSB(b, lo=0, hi=1024):
        return V(psum[b][:, :].bitcast(BF16)[:, lo:hi], [ps_b[b]])

    def bufs_of(vs):
        out = []
        for v in vs:
            out.extend(v.bufs)
        return out

    def mm(out, lhsT, rhs, start=True, stop=True):
        P.emit("pe", lambda e: e.matmul(out.ap, lhsT.ap, rhs.ap, start=start, stop=stop),
               reads=bufs_of([lhsT, rhs]), writes=out.bufs)

    def tr(out, in_, ident):
        P.emit("pe", lambda e: e.transpose(out.ap, in_.ap, ident.ap),
               reads=bufs_of([in_, ident]), writes=out.bufs)

    def act(out, in_, func, bias=None, scale=None, eng="act"):
        kw = {}
        rd = [in_]
        if bias is not None:
            if isinstance(bias, V):
                kw["bias"] = bias.ap
                rd.append(bias)
            else:
                kw["bias"] = bias
        if scale is not None:
            kw["scale"] = scale
        P.emit("act", lambda e: e.activation(out.ap, in_.ap, func, **kw), reads=bufs_of(rd), writes=out.bufs)

    def tt(out, a, b, op, eng="dve"):
        P.emit(eng, lambda e: e.tensor_tensor(out.ap, a.ap, b.ap, op), reads=bufs_of([a, b]), writes=out.bufs)

    def ts(out, a, s1, s2, op0, op1=None, eng="dve"):
        rd = [a]
        s1a = s1.ap if isinstance(s1, V) else s1
        s2a = s2.ap if isinstance(s2, V) else s2
        if isinstance(s1, V):
            rd.append(s1)
        if isinstance(s2, V):
            rd.append(s2)
        if op1 is None:
            P.emit(eng, lambda e: e.tensor_scalar(out.ap, a.ap, s1a, None, op0), reads=bufs_of(rd), writes=out.bufs)
        else:
            P.emit(eng, lambda e: e.tensor_scalar(out.ap, a.ap, s1a, s2a, op0, op1), reads=bufs_of(rd),
                   writes=out.bufs)

    def stt(out, a, s, b, op0, op1, eng="dve"):
        rd = [a, b]
        sa = s.ap if isinstance(s, V) else s
        if isinstance(s, V):
            rd.append(s)
        P.emit(eng, lambda e: e.scalar_tensor_tensor(out.ap, a.ap, sa, b.ap, op0, op1), reads=bufs_of(rd),
               writes=out.bufs)

    def cp(out, in_, eng="dve"):
        if eng == "act":
            P.emit("act", lambda e: e.copy(out.ap, in_.ap), reads=in_.bufs, writes=out.bufs)
        else:
            P.emit(eng, lambda e: e.tensor_copy(out.ap, in_.ap), reads=in_.bufs, writes=out.bufs)

    def rsq(out, in_):
        ts(out, in_, LN_EPS, None, ALU.add)
        act(out, out, AF.Ln)
        act(out, out, AF.Exp, scale=-0.5)

    def red(out, in_, op):
        P.emit("dve", lambda e: e.tensor_reduce(out.ap, in_.ap, AX.X, op), reads=in_.bufs, writes=out.bufs)

    def dma(q, out_ap, in_ap, reads, writes):
        P.emit(q, lambda e: e.dma_start(out=out_ap, in_=in_ap), reads=reads, writes=writes, dma=True)

    def ld(q, dst, src_ap, src_bufs):
        dma(q, dst.ap, src_ap, src_bufs, dst.bufs)

    def st(q, dst_ap, dst_bufs, src):
        dma(q, dst_ap, src.ap, src.bufs, dst_bufs)

    def bc(v, shape):
        return V(v.ap.unsqueeze(2).to_broadcast(shape), v.bufs)

    def split(v, a):
        return V(v.ap.rearrange("p (a b) -> p a b", a=a), v.bufs)

    def half(v, h):
        return V(v.ap[:, h * TH:(h + 1) * TH], v.bufs)

    def ppcol(name, j, n=1, part=128):
        o, _ = c.pp[name]
        return arena.view(PP_o + (o + j) * 4, [n], F32, part)

    slot_rr = [0]

    def wload(l, gi, name, j):
        o, KCm, J = wmeta[(l, gi, name)]
        n = 128 * KCm * 128
        src = wg_full[(l, gi)].ap().rearrange("r x -> (r x)")[o + j * n:o + (j + 1) * n].rearrange("(p y) -> p y", p=128)
        s = slot_rr[0] % NSLOT
        slot_rr[0] += 1
        dst = WR[s].row(0, 0, KCm * 128)
        ld("sp", dst, src, [wfull_b[(l, gi)]])
        return (lambda kc: WR[s].row(0, kc * 128, (kc + 1) * 128)), KCm

    def lin(l, gi, name, j, rhs_fn, banks):
        wv, KCm = wload(l, gi, name, j)
        for kc in range(KCm):
            for h in range(NH):
                mm(PS(banks[h], 0, TH), wv(kc), rhs_fn(kc, h), start=(kc == 0), stop=(kc == KCm - 1))

    def xt_rhs(kc, h):
        return XT.row(kc, h * TH, (h + 1) * TH)

    def opb_rhs(kc, h):
        return OPB.row(kc, h * TH, (h + 1) * TH)

    def opb_rhs_off(o):
        return lambda kc, h: OPB.row(o + kc, h * TH, (h + 1) * TH)

    ld("sp", CONST.all(), consts_in.ap().rearrange("p (a b) -> p a b", a=4), [])
    ld("sp", SEL.all(), sels_in.ap(), [])
    cp(IDB.all(), IDENT)
    RG = [list(range(NCORES))]
    for l in layers:
        for gi in range(len(_group_meta(c, l))):
            key = (l, gi)
            rows = wg_in[key].shape[0]
            step = 512
            for r0 in range(0, rows, step):
                r1 = min(rows, r0 + step)
                dma("pool", wg_sh[key].ap()[r0:r1, :], wg_in[key].ap()[r0:r1, :], [], [wsh_b[key]])
            P.emit("pool", (lambda k: (lambda e: e.collective_compute(
                "AllGather", ALU.bypass, replica_groups=RG, ins=[wg_sh[k].ap()], outs=[wg_full[k].ap()])))(key),
                reads=[wsh_b[key]], writes=[wfull_b[key]], dma=True, inc=1)

    for j in range(KC):
        tf = TF[j % 2]
        ld("sp", tf.row(0, 0, T), x_in.ap()[j], [XIN_b[j]])
        cp(XT.row(j), tf.row(0, 0, T), eng="act" if j % 2 else "dve")

    xres_src = (x_in, XIN_b)

    def layer_norm_finish(l, gname, bname, s1, s2, nfeat, last, router):
        for h in range(NH):
            mean = half(TF[0].row(0, 0, T), h)
            msq = half(TF[1].row(0, 0, T), h)
            ts(mean, PS(s1[h], 0, TH), 1.0 / nfeat, None, ALU.mult)
            tt(msq, mean, mean, ALU.mult)
            stt(msq, PS(s2[h], 0, TH), 1.0 / nfeat, msq, ALU.mult, ALU.subtract)
            rsq(half(RSTD.all(), h), msq)
            stt(half(NMR.all(), h), mean, -1.0, half(RSTD.all(), h), ALU.mult, ALU.mult)
        for b in s1 + s2:
            reserved.discard(b)
        lgb = None
        if router:
            lgb = [bank() for _ in range(NH)]
            for b in lgb:
                reserved.add(b)
        for j in range(KC):
            sv = TF[2 + (j % 2)].row(0, 0, T)
            ld("sp", sv, S.ap()[j], [S_b[j]])
            tt(sv, sv, RSTD.all(), ALU.mult)
            tt(sv, sv, NMR.all(), ALU.add)
            ts(sv, sv, ppcol(gname, j), ppcol(bname, j), ALU.mult, ALU.add)
            if last:
                st("pool", y_out.ap()[j], [YOUT_b[j]], sv)
            else:
                st("pool", XRES.ap()[j], [XRES_b[j]], sv)
                cp(XT.row(j), sv, eng="act")
            if router:
                o, _ = c.pp["wr"]
                wr = arena.view(PP_o + (o + j * c.NE) * 4, [c.NE], F32)
                for h in range(NH):
                    mm(PS(lgb[h], 0, TH, part=c.NE), wr, half(sv, h), start=(j == 0), stop=(j == KC - 1))
        return lgb

    def stats_accum(s1, s2, sv, sq, first, lastj):
        act(sq, sv, AF.Square)
        for h in range(NH):
            mm(PS(s1[h], 0, TH), ONES, half(sv, h), start=first, stop=lastj)
            mm(PS(s2[h], 0, TH), ONES, half(sq, h), start=first, stop=lastj)

    for li, l in enumerate(layers):
        is_last = (li == len(layers) - 1)
        moe = (l % 2 == 1)
        ld("sp", PPt.all(), pp_in[l].ap()[:, 0:NPL], [])
        o_wdt, _ = c.pp["wdt"]
        wtmp = arena.view(TF_o[0], [KC, c.H], F32)
        ld("sp", wtmp, pp_in[l].ap()[:, o_wdt:o_wdt + KC * c.H].rearrange("p (a b) -> p a b", a=KC), [])
        cp(WDTB.all(), wtmp)
        act(AN.all(), ppcol("a_log", 0, c.H), AF.Exp)
        ts(AN.all(), AN.all(), -1.0, None, ALU.mult)

        for j in range(c.CCc):
            ba = [bank() for _ in range(NH)]
            bg = [bank() for _ in range(NH)]
            lin(l, 0, "glu_a", j, xt_rhs, ba)
            lin(l, 0, "glu_g", j, xt_rhs, bg)
            sg = TF[(j % 2) * 2].row(0, 0, T)
            uu = TF[(j % 2) * 2 + 1].row(0, 0, T)
            for h in range(NH):
                act(half(sg, h), PS(bg[h], 0, TH), AF.Sigmoid)
                tt(half(uu, h), PS(ba[h], 0, TH), half(sg, h), ALU.mult)
            st("pool", U.ap()[j], [U_b[j]], uu)
            st("pool", HSEND.ap()[:, j * 30:(j + 1) * 30], [HSEND_b], TF[(j % 2) * 2 + 1].row(0, T - 30, T))

        for j in range(c.XBCc):
            bx = [bank() for _ in range(NH)]
            lin(l, 0, "xbc", j, xt_rhs, bx)
            xx = TF[j % 4].row(0, 0, T)
            for h in range(NH):
                cp(half(xx, h), PS(bx[h], 0, TH), eng="act")
            st("pool", XB.ap()[j], [XB_b[j]], xx)
            st("pool", HSEND.ap()[:, c.CCc * 30 + j * 3:c.CCc * 30 + (j + 1) * 3], [HSEND_b],
               TF[j % 4].row(0, T - 3, T))
        for j in range(c.DIc):
            bz = [bank() for _ in range(NH)]
            lin(l, 0, "z", j, xt_rhs, bz)
            zz = TB[j % 4].all()
            for h in range(NH):
                act(half(zz, h), PS(bz[h], 0, TH), AF.Silu)
            st("pool", ZS.ap()[j], [ZS_b[j]], zz)
        for j in range(2 * KC):
            bq = [bank() for _ in range(NH)]
            lin(l, 0, "gates", j, xt_rhs, bq)
            gg = TB[j % 4].all()
            for h in range(NH):
                act(half(gg, h), PS(bq[h], 0, TH), AF.Sigmoid, bias=ppcol("b_gate", j))
            st("pool", GT.ap()[j], [GT_b[j]], gg)
        H = c.H
        for t in range(NT):
            b = bank()
            for kc in range(KC):
                mm(PS(b, 0, H), XT.row(kc, t * 128, (t + 1) * 128), WDTB.row(kc), start=(kc == 0), stop=(kc == KC - 1))
            xv = SMt["TMP"].row(t)
            av = SMt["TMP2"].row(t)
            tt(xv, PS(b, 0, H), ppcol("dt_bias", 0, H), ALU.add)
            stt(av, xv, -1.0, xv, ALU.mult, ALU.max)
            act(av, av, AF.Exp, scale=-1.0)
            ts(av, av, 1.0, None, ALU.add)
            act(av, av, AF.Ln)
            stt(SMt["DT"].row(t), xv, 0.0, av, ALU.max, ALU.add)
            tt(SMt["DA"].row(t), SMt["DT"].row(t), AN.all(), ALU.mult)

        P.emit("pool", lambda e: e.collective_compute("AllGather", ALU.bypass, replica_groups=RG,
                                                      ins=[HSEND.ap()], outs=[HG.ap()]),
               reads=[HSEND_b], writes=[HG_b], dma=True, inc=1)
        for r in range(NCORES):
            hl = HL[r % 2].all()
            ld("sp", hl, HG.ap()[r * 128:(r + 1) * 128, :], [HG_b])
            if r == 0:
                ts(HA.all(), hl, SEL.row(0, 0, 1), None, ALU.mult)
            else:
                stt(HA.all(), hl, SEL.row(0, r, r + 1), HA.all(), ALU.mult, ALU.add)

        o_sw, _ = c.pp["ssm_conv_w"]
        for j in range(c.XBCc):
            tx = TF[j % 2]
            ld("sp", tx.row(0, 3, 3 + T), XB.ap()[j], [XB_b[j]])
            cp(tx.row(0, 0, 3), HA.row(0, c.CCc * 30 + j * 3, c.CCc * 30 + (j + 1) * 3))
            acc = TF[2 + (j % 2)].row(0, 0, T)
            for k in range(c.SK):
                w = arena.view(PP_o + (o_sw + j * c.SK + k) * 4, [1], F32)
                if k == 0:
                    ts(acc, tx.row(0, k, k + T), w, None, ALU.mult)
                else:
                    stt(acc, tx.row(0, k, k + T), w, acc, ALU.mult, ALU.add)
            if j < c.DIc:
                xb = TB[j % 2].all()
                act(xb, acc, AF.Silu, bias=ppcol("ssm_conv_b", j))
                for t0 in range(0, NT, 4):
                    b = bank()
                    nt4 = min(4, NT - t0)
                    for tq in range(nt4):
                        tr(PSB(b, tq * 128, (tq + 1) * 128), TB[j % 2].row(0, (t0 + tq) * 128, (t0 + tq + 1) * 128),
                           IDB.all())
                    dst = V(XSTM.all().ap[:, t0:t0 + nt4, j * 128:(j + 1) * 128], XSTM.rows(t0, t0 + nt4).bufs)
                    cp(dst, split(PSB(b, 0, nt4 * 128), nt4))
            elif j < c.DIc + c.G:
                g = j - c.DIc
                act(BFM.row(g), acc, AF.Silu, bias=ppcol("ssm_conv_b", j))
                for t0 in range(0, NT, 4):
                    b = bank()
                    nt4 = min(4, NT - t0)
                    for tq in range(nt4):
                        tr(PSB(b, tq * 128, (tq + 1) * 128), BFM.row(g, (t0 + tq) * 128, (t0 + tq + 1) * 128), IDB.all())
                    dst = V(BTM.all().ap[:, t0:t0 + nt4, g * 128:(g + 1) * 128], BTM.rows(t0, t0 + nt4).bufs)
                    cp(dst, split(PSB(b, 0, nt4 * 128), nt4))
            else:
                g = j - c.DIc - c.G
                act(CFM.row(g), acc, AF.Silu, bias=ppcol("ssm_conv_b", j))

        E, EW = c.E, c.EW
        for t in range(NT):
            b = bank()
            mm(PS(b, 0, H), TRI, SMt["DA"].row(t))
            mm(PS(b, H, 2 * H), ONES, SMt["DA"].row(t))
            cp(SMt["CS"].row(t), PS(b, 0, H))
            cp(SMt["TOT"].row(t), PS(b, H, 2 * H))
            act(SMt["E1"].row(t), SMt["CS"].row(t), AF.Exp)
            act(SMt["ACH"].row(t), SMt["TOT"].row(t), AF.Exp)
            tt(SMt["W2"].row(t), SMt["TOT"].row(t), SMt["CS"].row(t), ALU.subtract)
            act(SMt["W2"].row(t), SMt["W2"].row(t), AF.Exp)
            tt(SMt["W2"].row(t), SMt["W2"].row(t), SMt["DT"].row(t), ALU.mult)
            if t == 0:
                cp(LTOT.all(), SMt["TOT"].row(t))
            else:
                tt(LTOT.all(), LTOT.all(), SMt["TOT"].row(t), ALU.add)

        def chunk_state(g, t, b):
            xw = XW[t % 2].all()
            tt(split(xw, E), split(XSTM.row(t, g * EW, (g + 1) * EW), E),
               bc(SMt["W2"].row(t, g * E, (g + 1) * E), [128, E, 64]), ALU.mult)
            mm(PS(b, 0, EW), BTM.row(t, g * 128, (g + 1) * 128), xw)

        for g in range(c.G):
            for t in range(NT):
                b = bank()
                chunk_state(g, t, b)
                if t == 0:
                    cp(STL.row(g), PS(b, 0, EW))
                else:
                    tt(split(STL.row(g), E), split(STL.row(g), E),
                       bc(SMt["ACH"].row(t, g * E, (g + 1) * E), [128, E, 64]), ALU.mult)
                    tt(STL.row(g), STL.row(g), PS(b, 0, EW), ALU.add)
        st("pool", SSEND.ap()[:, 0:c.G * EW], [SSEND_b], V(STL.all().ap.rearrange("p a b -> p (a b)"), STL.all().bufs))
        st("pool", SSEND.ap()[:, c.G * EW:c.SF], [SSEND_b], LTOT.all())
        P.emit("pool", lambda e: e.collective_compute("AllGather", ALU.bypass, replica_groups=RG,
                                                      ins=[SSEND.ap()], outs=[SG.ap()]),
               reads=[SSEND_b], writes=[SG_b], dma=True, inc=1)

        o_dsk, _ = c.pp["d_skip"]
        for g in range(c.G):
            for r in range(NCORES):
                fl = FL[r % 2]
                ld("sp", fl.row(0, 0, EW), SG.ap()[r * 128:(r + 1) * 128, g * EW:(g + 1) * EW], [SG_b])
                ld("sp", fl.row(0, EW, EW + c.H), SG.ap()[r * 128:(r + 1) * 128, c.G * EW:c.SF], [SG_b])
                if r == 0:
                    P.emit("dve", (lambda v: (lambda e: e.memset(v.ap, 0.0)))(RR.all()), writes=RR.all().bufs)
                    P.emit("dve", (lambda v: (lambda e: e.memset(v.ap, 0.0)))(STT.all()), writes=STT.all().bufs)
                else:
                    stt(STT.all(), RR.all(), SEL.row(0, 8 + r, 9 + r), STT.all(), ALU.mult, ALU.add)
                if r < NCORES - 1:
                    ea = fl.row(0, EW + g * E, EW + (g + 1) * E)
                    act(ea, ea, AF.Exp)
                    tt(split(RR.all(), E), split(RR.all(), E), bc(ea, [128, E, 64]), ALU.mult)
                    tt(RR.all(), RR.all(), fl.row(0, 0, EW), ALU.add)
            for t in range(NT):
                tcols = (t * 128, (t + 1) * 128)
                bs = bank()
                mm(PS(bs, 0, 128), BFM.row(g, *tcols), CFM.row(g, *tcols))
                tt(SMK.all(), PS(bs, 0, 128), TRI, ALU.mult)
                by = bank()
                for h0 in range(0, E, 4):
                    nh4 = min(4, E - h0)
                    lh = LH[(h0 // 4) % 2]
                    dd = DD[(h0 // 4) % 2]
                    mh = MH[(h0 // 4) % 2]
                    bd = bank()
                    for q in range(nh4):
                        hh = g * E + h0 + q
                        ts(lh.row(q), GTM, SMt["DA"].row(t, hh, hh + 1), None, ALU.mult)
                        mm(PS(bd, q * 128, (q + 1) * 128), lh.row(q), TRI)
                    act(dd.rows(0, nh4), split(PS(bd, 0, nh4 * 128), nh4), AF.Exp)
                    for q in range(nh4):
                        hh = g * E + h0 + q
                        stt(mh.row(q), dd.row(q), SMt["DT"].row(t, hh, hh + 1), SMK.all(), ALU.mult, ALU.mult)
                        mm(PS(by, (h0 + q) * 64, (h0 + q + 1) * 64), mh.row(q),
                           XSTM.row(t, hh * 64, (hh + 1) * 64))
                cp(STB.all(), STT.all(), eng="act")
                bo = bank()
                mm(PS(bo, 0, EW), CFM.row(g, *tcols), STB.all())
                yt = YT[t % 2].all()
                tt(split(yt, E), split(PS(bo, 0, EW), E), bc(SMt["E1"].row(t, g * E, (g + 1) * E), [128, E, 64]),
                   ALU.mult)
                tt(yt, yt, PS(by, 0, EW), ALU.add)
                dsk = arena.view(PP_o + (o_dsk + g * E) * 4, [E], F32)
                tt(split(YT2.all(), E), split(XSTM.row(t, g * EW, (g + 1) * EW), E), bc(dsk, [128, E, 64]), ALU.mult)
                tt(yt, yt, YT2.all(), ALU.add)
                if t < NT - 1:
                    b2 = bank()
                    chunk_state(g, t, b2)
                    tt(split(STT.all(), E), split(STT.all(), E),
                       bc(SMt["ACH"].row(t, g * E, (g + 1) * E), [128, E, 64]), ALU.mult)
                    tt(STT.all(), STT.all(), PS(b2, 0, EW), ALU.add)
                bt = bank()
                for i in range(c.EWc):
                    tr(PS(bt, i * 128, (i + 1) * 128), YT[t % 2].row(0, i * 128, (i + 1) * 128), IDENT)
                dst = V(YF.all().ap[:, :, t * 128:(t + 1) * 128], YF.all().bufs)
                cp(dst, split(PS(bt, 0, c.EWc * 128), c.EWc), eng="act")
            sb = [bank() for _ in range(NH)]
            for b in sb:
                reserved.add(b)
            for i in range(c.EWc):
                zt = TB[i % 4].all()
                ld("sp", zt, ZS.ap()[g * c.EWc + i], [ZS_b[g * c.EWc + i]])
                tt(YF.row(i), YF.row(i), zt, ALU.mult)
                sq = TF[i % 4].row(0, 0, T)
                act(sq, YF.row(i), AF.Square)
                for h in range(NH):
                    mm(PS(sb[h], 0, TH), ONES, half(sq, h), start=(i == 0), stop=(i == c.EWc - 1))
            for h in range(NH):
                ts(half(RSTD.all(), h), PS(sb[h], 0, TH), 1.0 / EW, None, ALU.mult)
                rsq(half(RSTD.all(), h), half(RSTD.all(), h))
            for b in sb:
                reserved.discard(b)
            for i in range(c.EWc):
                stt(OPB.row(c.CCc + g * c.EWc + i), YF.row(i), ppcol("ssm_norm_g", g * c.EWc + i), RSTD.all(),
                    ALU.mult, ALU.mult)

        o_cw, _ = c.pp["conv_dw"]
        s1 = [bank() for _ in range(NH)]
        s2 = [bank() for _ in range(NH)]
        for b in s1 + s2:
            reserved.add(b)
        for j in range(c.CCc):
            tu = TF[j % 2]
            ld("sp", tu.row(0, 30, 30 + T), U.ap()[j], [U_b[j]])
            cp(tu.row(0, 0, 30), HA.row(0, j * 30, (j + 1) * 30), eng="act")
            acc = TF[2].row(0, 0, T)
            for k in range(c.CK):
                w = arena.view(PP_o + (o_cw + j * c.CK + k) * 4, [1], F32)
                if k == 0:
                    ts(acc, tu.row(0, k, k + T), w, None, ALU.mult)
                else:
                    stt(acc, tu.row(0, k, k + T), w, acc, ALU.mult, ALU.add)
            stats_accum(s1, s2, acc, TF[3].row(0, 0, T), j == 0, j == c.CCc - 1)
            st("pool", U.ap()[j], [U_b[j]], acc)
        for h in range(NH):
            mean = half(TF[0].row(0, 0, T), h)
            msq = half(TF[1].row(0, 0, T), h)
            ts(mean, PS(s1[h], 0, TH), 1.0 / c.CC, None, ALU.mult)
            tt(msq, mean, mean, ALU.mult)
            stt(msq, PS(s2[h], 0, TH), 1.0 / c.CC, msq, ALU.mult, ALU.subtract)
            rsq(half(RSTD.all(), h), msq)
            stt(half(NMR.all(), h), mean, -1.0, half(RSTD.all(), h), ALU.mult, ALU.mult)
        for b in s1 + s2:
            reserved.discard(b)
        for j in range(c.CCc):
            uv = TF[2 + (j % 2)].row(0, 0, T)
            ld("sp", uv, U.ap()[j], [U_b[j]])
            tt(uv, uv, RSTD.all(), ALU.mult)
            tt(uv, uv, NMR.all(), ALU.add)
            ts(uv, uv, ppcol("conv_ln_g", j), ppcol("conv_ln_b", j), ALU.mult, ALU.add)
            act(OPB.row(j), uv, AF.Silu)

        for j in range(KC):
            bcb = [bank() for _ in range(NH)]
            bsb = [bank() for _ in range(NH)]
            lin(l, 1, "pw2", j, opb_rhs, bcb)
            lin(l, 1, "ssm_out", j, opb_rhs_off(c.CCc), bsb)
            gc = TB[0].all()
            gs = TB[1].all()
            ld("sp", gc, GT.ap()[j], [GT_b[j]])
            ld("sp", gs, GT.ap()[KC + j], [GT_b[KC + j]])
            ta = TF[0].row(0, 0, T)
            tb2 = TF[1].row(0, 0, T)
            mb = TB[2 + (j % 2)].all()
            for h in range(NH):
                tt(half(ta, h), PS(bcb[h], 0, TH), half(gc, h), ALU.mult)
                tt(half(tb2, h), PS(bsb[h], 0, TH), half(gs, h), ALU.mult)
                tt(half(mb, h), half(ta, h), half(tb2, h), ALU.add)
            st("pool", MRG.ap()[j], [MRG_b[j]], mb)
        for j in range(KC):
            ld("sp", OPB.row(j), MRG.ap()[j], [MRG_b[j]])

        s1 = [bank() for _ in range(NH)]
        s2 = [bank() for _ in range(NH)]
        for b in s1 + s2:
            reserved.add(b)
        for j in range(KC):
            bm = [bank() for _ in range(NH)]
            lin(l, 1, "w_o", j, opb_rhs, bm)
            xo = TF[j % 2].row(0, 0, T)
            ld("sp", xo, xres_src[0].ap()[j], [xres_src[1][j]])
            for h in range(NH):
                stt(half(xo, h), half(xo, h), c.ALPHA, PS(bm[h], 0, TH), ALU.mult, ALU.add)
            stats_accum(s1, s2, xo, TF[2 + (j % 2)].row(0, 0, T), j == 0, j == KC - 1)
            st("pool", S.ap()[j], [S_b[j]], xo)
        xres_src = (XRES, XRES_b)
        lgb = layer_norm_finish(l, "ln_mix_g", "ln_mix_b", s1, s2, c.D, False, moe)

        NE = c.NE
        if moe:
            for h in range(NH):
                cp(V(LG.all().ap[:, h * TH:(h + 1) * TH], LG.all().bufs), PS(lgb[h], 0, TH, part=NE), eng="act")
            for b in lgb:
                reserved.discard(b)
            idn = V(CONST.row(0).ap[0:NE, 0:NE], CONST.row(0).bufs)
            for t in range(NT):
                b = bank()
                tr(PS(b, 0, NE), V(LG.all().ap[:, t * 128:(t + 1) * 128], LG.all().bufs), idn)
                cp(RTt["LT"].row(t), PS(b, 0, NE))
            LTv, EQ, L2, EX, CMB = (RTt[k].all() for k in ("LT", "EQ", "L2", "EX", "CMB"))
            M1, M2, DEN = (RMt[k].all() for k in ("M1", "M2", "DEN"))
            red(M1, LTv, ALU.max)
            tt(EQ, LTv, bc(M1, [128, NT, NE]), ALU.is_equal)
            stt(L2, EQ, -1e30, LTv, ALU.mult, ALU.add)
            red(M2, L2, ALU.max)
            tt(EQ, LTv, bc(M2, [128, NT, NE]), ALU.is_ge)
            tt(EX, LTv, bc(M1, [128, NT, NE]), ALU.subtract)
            act(EX, EX, AF.Exp)
            tt(EX, EX, EQ, ALU.mult)
            red(DEN, EX, ALU.add)
            P.emit("dve", lambda e: e.reciprocal(DEN.ap, DEN.ap), reads=DEN.bufs, writes=DEN.bufs)
            tt(CMB, EX, bc(DEN, [128, NT, NE]), ALU.mult)

        groups = c.ffn_groups(l)
        for gq, grp in enumerate(groups):
            gi = 2 + gq
            firstg = (gq == 0)
            lastg = (gq == len(groups) - 1)
            jj = 0
            for (kind, ex, a, b_) in grp:
                cmb_t = None
                if kind == "e":
                    cmb_t = TF[2 + (ex % 2)].row(0, 0, T)
                    cb = [bank() for _ in range(NH)]
                    for t in range(NT):
                        dg = DG[t % 2].all()
                        ts(dg, IDENT, RTt["CMB"].row(t, ex, ex + 1), None, ALU.mult)
                        hh_, col = divmod(t * 128, TH)
                        mm(PS(cb[hh_], col, col + 128), ONES, dg)
                    for h in range(NH):
                        cp(half(cmb_t, h), PS(cb[h], 0, TH), eng="act")
                for ch in range(a, b_):
                    bg_ = [bank() for _ in range(NH)]
                    bu_ = [bank() for _ in range(NH)]
                    lin(l, gi, "fg", jj, xt_rhs, bg_)
                    lin(l, gi, "fu", jj, xt_rhs, bu_)
                    sgt = TF[jj % 2].row(0, 0, T)
                    for h in range(NH):
                        act(half(sgt, h), PS(bg_[h], 0, TH), AF.Silu)
                        if cmb_t is None:
                            tt(half(OPB.row(jj), h), half(sgt, h), PS(bu_[h], 0, TH), ALU.mult)
                        else:
                            tt(half(sgt, h), half(sgt, h), PS(bu_[h], 0, TH), ALU.mult)
                            tt(half(OPB.row(jj), h), half(sgt, h), half(cmb_t, h), ALU.mult)
                    jj += 1
            if lastg:
                s1 = [bank() for _ in range(NH)]
                s2 = [bank() for _ in range(NH)]
                for b in s1 + s2:
                    reserved.add(b)
            for j in range(KC):
                bm = [bank() for _ in range(NH)]
                lin(l, gi, "fd", j, opb_rhs, bm)
                xo = TF[j % 2].row(0, 0, T)
                if firstg:
                    ld("sp", xo, XRES.ap()[j], [XRES_b[j]])
                    for h in range(NH):
                        stt(half(xo, h), half(xo, h), c.ALPHA, PS(bm[h], 0, TH), ALU.mult, ALU.add)
                else:
                    ld("sp", xo, S.ap()[j], [S_b[j]])
                    for h in range(NH):
                        tt(half(xo, h), half(xo, h), PS(bm[h], 0, TH), ALU.add)
                if lastg:
                    stats_accum(s1, s2, xo, TF[2 + (j % 2)].row(0, 0, T), j == 0, j == KC - 1)
                st("pool", S.ap()[j], [S_b[j]], xo)
        layer_norm_finish(l, "ln_ffn_g", "ln_ffn_b", s1, s2, c.D, is_last, False)

    P.finalize(stack)
    P.run_block()
    return nc, stack


def make_in_maps(cfg, inp, layers=None):
    c = cfg
    if layers is None:
        layers = list(range(c.DEPTH))
    x = np.asarray(inp["x"], np.float32).reshape(c.SEQ, c.D)
    consts = _consts()
    maps = [dict() for _ in range(NCORES)]
    for r in range(NCORES):
        xr = x[r * c.T:(r + 1) * c.T]
        maps[r]["x_in"] = np.ascontiguousarray(xr.T).reshape(c.KC, 128, c.T)
        maps[r]["consts"] = consts
        sel = np.zeros((128, 16), np.float32)
        if r > 0:
            sel[:, r - 1] = 1.0
        sel[:, 8 + r] = 1.0
        maps[r]["sels"] = sel
    inp = {k: np.asarray(v, np.float32) for k, v in inp.items()}
    for l in layers:
        pp = _pack_params(c, l, inp)
        for r in range(NCORES):
            maps[r]["pp_%d" % l] = pp
        for gi, mats in enumerate(_layer_weight_groups(c, l, inp)):
            flat = np.concatenate([_wl(W) for (_, W) in mats])
            n = flat.size // NCORES
            for r in range(NCORES):
                maps[r]["wg_%d_%d" % (l, gi)] = flat[r * n:(r + 1) * n].reshape(-1, 2048)
    return maps


def run(cfg, inp, layers=None):
    nc, stack = build(cfg, layers)
    with stack:
        maps = make_in_maps(cfg, inp, layers)
        res = run_bass_kernel_spmd(nc, maps, core_ids=list(range(NCORES)))
    outs = [np.asarray(r["y_out"]).reshape(cfg.D, cfg.T).T for r in res.results]
    return np.concatenate(outs, axis=0).reshape(1, cfg.SEQ, cfg.D).astype(np.float32)


def kernel(**inputs):
    return run(Cfg(), inputs)
```
